# Optimizing a Trainium2 kernel written in Bass

```python
import jax, jax.numpy as jnp
from jax import lax
import numpy as np

D_MODEL = 2048
BATCH = 8
SEQ = 4096
DEPTH = 2
DEC_BATCH = 4
DEC_SEQ = 8192
PAST_LEN = 128

HEAD_DIM = 64
MIX_WIDTH = D_MODEL
RW_WIDTH = 3 * MIX_WIDTH // 8
RW_HEADS = RW_WIDTH // HEAD_DIM
RW_DECAY_RANK = 64
RW_ICLR_RANK = 64
RW_GATE_RANK = 128
RW_COLS = 3 * RW_WIDTH + RW_DECAY_RANK + RW_ICLR_RANK + RW_GATE_RANK
RW_GN_EPS = 64e-5
ATT_WIDTH = 3 * MIX_WIDTH // 8
ATT_Q_HEADS = ATT_WIDTH // HEAD_DIM
ATT_KV_HEADS = 4
ATT_GROUP = ATT_Q_HEADS // ATT_KV_HEADS
ATT_KV_WIDTH = ATT_KV_HEADS * HEAD_DIM
ATT_COLS = ATT_WIDTH + 2 * ATT_KV_WIDTH
WINDOW = 128
ATT_BLOCK = 128
LRU_WIDTH = MIX_WIDTH - RW_WIDTH - ATT_WIDTH
LRU_BLOCKS = 8
LRU_BLOCK_DIM = LRU_WIDTH // LRU_BLOCKS
LRU_COLS = 2 * LRU_WIDTH
CONV_WIDTH = 4
CONV_LEFT = 2
LRU_C = 8.0
IN_WIDTH = RW_COLS + ATT_COLS + LRU_COLS
N_EXPERTS = 16
N_GROUPS = 4
EXPERTS_PER_GROUP = N_EXPERTS // N_GROUPS
TOP_K = 2
D_FF_EXPERT = D_MODEL // 2
MOE_BLOCK = 512
ALPHA = (2.0 * DEPTH) ** 0.25
BETA = (8.0 * DEPTH) ** -0.25
LN_EPS = 1e-5

kernel_name = 'hybrid_bidir_encoder'


def _layer_norm(x, g, b):
    xf = x.astype(jnp.float32)
    mu = jnp.mean(xf, -1, keepdims=True)
    var = jnp.mean(jnp.square(xf - mu), -1, keepdims=True)
    return ((xf - mu) * lax.rsqrt(var + LN_EPS) * g + b).astype(x.dtype)


def _heads(t):
    return t.reshape(t.shape[:-1] + (RW_HEADS, HEAD_DIM))


def _centred_shift(p):
    pp = jnp.pad(p, ((0, 0), (1, 1), (0, 0)))
    return 0.5 * (pp[:, :-2] + pp[:, 2:])


def _both_dirs(t):
    return jnp.stack([t, jnp.flip(t, 1)])


def _flip_bwd(t):
    return jnp.stack([t[0], jnp.flip(t[1], 1)])


def _rwkv7_step(S, inp):
    r_t, kk_t, v_t, k_t, w_t, a_t = inp
    s_kk = jnp.einsum('dbhvk,dbhk->dbhv', S, -kk_t)
    S = (S * w_t[..., None, :]
         + s_kk[..., :, None] * (kk_t * a_t)[..., None, :]
         + v_t[..., :, None] * k_t[..., None, :])
    y = jnp.einsum('dbhvk,dbhk->dbhv', S, r_t)
    return S, y


def _rwkv7(p, mu, w0, w_up, a0, a_up, g_up, k_k, k_a, r_k, lnx_g, lnx_b):
    B, T, _ = p.shape
    p = p + mu * (_centred_shift(p) - p)
    o3 = 3 * RW_WIDTH
    o4 = o3 + RW_DECAY_RANK
    o5 = o4 + RW_ICLR_RANK
    r, k, v, wd, ad, gd = jnp.split(p, [RW_WIDTH, 2 * RW_WIDTH, o3, o4, o5], axis=-1)
    log_w = -jax.nn.softplus(-(w0[:, None, None, :] + jnp.einsum('btr,drc->dbtc', jnp.tanh(wd), w_up))) - 0.5
    w = jnp.exp(-jnp.exp(log_w))
    a = jax.nn.sigmoid(a0[:, None, None, :] + jnp.einsum('btr,drc->dbtc', ad, a_up))
    g = jnp.einsum('btr,rc->btc', jax.nn.sigmoid(gd), g_up)
    kkf = _heads(k * k_k).astype(jnp.float32)
    kk = (kkf * lax.rsqrt(jnp.maximum(jnp.sum(kkf * kkf, -1, keepdims=True), 1e-24))).astype(p.dtype)
    k_mod = k[None] * (1.0 + (a - 1.0) * k_a)
    rr, vv = _heads(r), _heads(v)
    seqs = (_both_dirs(rr), _both_dirs(kk), _both_dirs(vv),
            _flip_bwd(_heads(k_mod)), _flip_bwd(_heads(w)), _flip_bwd(_heads(a)))
    seqs = tuple(jnp.moveaxis(s, 2, 0) for s in seqs)
    S0 = jnp.zeros((2, B, RW_HEADS, HEAD_DIM, HEAD_DIM), p.dtype)
    _, ys = lax.scan(_rwkv7_step, S0, seqs)
    ys = jnp.moveaxis(ys, 0, 2)
    y = ys[0] + jnp.flip(ys[1], 1)
    yf = y.astype(jnp.float32)
    m = jnp.mean(yf, -1, keepdims=True)
    var = jnp.mean(jnp.square(yf - m), -1, keepdims=True)
    y = ((yf - m) * lax.rsqrt(var + RW_GN_EPS)).reshape(B, T, RW_WIDTH)
    y = (y * lnx_g + lnx_b).astype(p.dtype)
    k_bar = _heads(0.5 * (k_mod[0] + k_mod[1]))
    bonus = (jnp.sum(rr * k_bar * r_k, -1, keepdims=True) * vv).reshape(B, T, RW_WIDTH)
    return (y + bonus) * g


def _alibi_slopes():
    return 2.0 ** (-8.0 * jnp.arange(1, ATT_Q_HEADS + 1, dtype=jnp.float32) / ATT_Q_HEADS)


def _window_attention(p, sink):
    B, T, _ = p.shape
    nb = T // ATT_BLOCK
    q, k, v = jnp.split(p, [ATT_WIDTH, ATT_WIDTH + ATT_KV_WIDTH], axis=-1)
    qb = q.reshape(B, nb, ATT_BLOCK, ATT_KV_HEADS, ATT_GROUP, HEAD_DIM)

    def bands(t):
        tp = jnp.pad(t.reshape(B, T, ATT_KV_HEADS, HEAD_DIM), ((0, 0), (ATT_BLOCK, ATT_BLOCK), (0, 0), (0, 0)))
        tp = tp.reshape(B, nb + 2, ATT_BLOCK, ATT_KV_HEADS, HEAD_DIM)
        return jnp.concatenate([tp[:, :-2], tp[:, 1:-1], tp[:, 2:]], axis=2)

    kb, vb = bands(k), bands(v)
    s = jnp.einsum('bnqhgd,bnkhd->bnhgqk', qb, kb).astype(jnp.float32) * (HEAD_DIM ** -0.5)
    q_off = jnp.arange(ATT_BLOCK)
    k_off = jnp.arange(3 * ATT_BLOCK) - ATT_BLOCK
    dist_i = jnp.abs(q_off[:, None] - k_off[None, :])
    k_abs = jnp.arange(nb)[:, None] * ATT_BLOCK + k_off[None, :]
    mask = (dist_i <= WINDOW)[None] & ((k_abs >= 0) & (k_abs < T))[:, None, :]
    slopes = _alibi_slopes().reshape(ATT_KV_HEADS, ATT_GROUP)
    s = s - slopes[:, :, None, None] * dist_i.astype(jnp.float32)
    s = jnp.where(mask[None, :, None, None], s, -jnp.inf)
    sink_f = sink.astype(jnp.float32).reshape(ATT_KV_HEADS, ATT_GROUP)[:, :, None]
    m = jnp.maximum(jnp.max(s, -1), sink_f)
    e = jnp.exp(s - m[..., None])
    denom = jnp.sum(e, -1) + jnp.exp(sink_f - m)
    probs = (e / denom[..., None]).astype(p.dtype)
    o = jnp.einsum('bnhgqk,bnkhd->bnqhgd', probs, vb)
    return o.reshape(B, T, ATT_WIDTH)


def _lin_combine(c1, c2):
    a1, b1 = c1
    a2, b2 = c2
    return a1 * a2, a2 * b1 + b2


def _rglru(p, conv_w, conv_b, wa, ba, wx, bx, lam):
    B, T, _ = p.shape
    xb, gate = jnp.split(p, [LRU_WIDTH], axis=-1)
    xp = jnp.pad(xb, ((0, 0), (CONV_LEFT, CONV_WIDTH - 1 - CONV_LEFT), (0, 0)))
    xc = conv_b + sum(xp[:, j:j + T] * conv_w[j] for j in range(CONV_WIDTH))
    xcb = xc.reshape(B, T, LRU_BLOCKS, LRU_BLOCK_DIM)
    gr = jnp.einsum('btnc,dncf->dbtnf', xcb, wa).reshape(2, B, T, LRU_WIDTH) + ba[:, None, None, :]
    gi = jnp.einsum('btnc,dncf->dbtnf', xcb, wx).reshape(2, B, T, LRU_WIDTH) + bx[:, None, None, :]
    log_a = -LRU_C * jax.nn.sigmoid(gr) * jax.nn.softplus(-lam)[:, None, None, :]
    a = jnp.exp(log_a)
    b = jnp.sqrt(-jnp.expm1(2.0 * log_a)) * jax.nn.sigmoid(gi) * xc[None]
    h_f = lax.associative_scan(_lin_combine, (a[0], b[0]), axis=1)[1]
    h_b = lax.associative_scan(_lin_combine, (a[1], b[1]), reverse=True, axis=1)[1]
    return (h_f + h_b) * jax.nn.gelu(gate)


def _moe(x, router_w, router_bias, w1, w3, w2):
    B, T, D = x.shape
    n_tok = B * T
    xt = x.reshape(n_tok, D)
    scores = jax.nn.sigmoid(jnp.einsum('nd,de->ne', xt, router_w).astype(jnp.float32))
    sel = scores + router_bias.astype(jnp.float32)
    grp = jnp.sum(lax.top_k(sel.reshape(n_tok, N_GROUPS, EXPERTS_PER_GROUP), TOP_K)[0], -1)
    best = jnp.argmax(grp, axis=-1)
    in_grp = (jnp.arange(N_EXPERTS) // EXPERTS_PER_GROUP)[None, :] == best[:, None]
    _, idx = lax.top_k(jnp.where(in_grp, sel, -jnp.inf), TOP_K)
    s_top = jnp.take_along_axis(scores, idx, axis=-1)
    gates = s_top / jnp.sum(s_top, -1, keepdims=True)
    n_assign = n_tok * TOP_K
    flat_e = idx.reshape(-1)
    flat_tok = jnp.repeat(jnp.arange(n_tok, dtype=jnp.int32), TOP_K)
    flat_gate = gates.reshape(-1)
    order = jnp.argsort(flat_e)
    e_sorted = flat_e[order]
    counts = jnp.bincount(flat_e, length=N_EXPERTS)
    padded = (counts + MOE_BLOCK - 1) // MOE_BLOCK * MOE_BLOCK
    pad_end = jnp.cumsum(padded)
    pad_start = pad_end - padded
    start = jnp.cumsum(counts) - counts
    dest = pad_start[e_sorted] + jnp.arange(n_assign) - start[e_sorted]
    n_blocks = -(-n_assign // MOE_BLOCK) + N_EXPERTS
    cap = n_blocks * MOE_BLOCK
    row_tok = jnp.zeros((cap,), jnp.int32).at[dest].set(flat_tok[order])
    row_gate = jnp.zeros((cap,), jnp.float32).at[dest].set(flat_gate[order])
    block_e = jnp.minimum(jnp.searchsorted(pad_end, jnp.arange(n_blocks) * MOE_BLOCK, side='right'), N_EXPERTS - 1)
    xs = xt[row_tok].reshape(n_blocks, MOE_BLOCK, D)

    def expert_block(args):
        xb, e = args
        h = jax.nn.silu(xb @ w1[e]) * (xb @ w3[e])
        return h @ w2[e]

    ys = lax.map(expert_block, (xs, block_e)).reshape(cap, D)
    ys = ys * row_gate[:, None].astype(x.dtype)
    return jax.ops.segment_sum(ys, row_tok, num_segments=n_tok).reshape(B, T, D)


def _trunk(x, w_in, rw_mu, rw_w0, rw_w_up, rw_a0, rw_a_up, rw_g_up, rw_k_k, rw_k_a, rw_r_k,
           rw_lnx_g, rw_lnx_b, att_sink, lru_conv_w, lru_conv_b, lru_wa, lru_ba, lru_wx, lru_bx,
           lru_lambda, w_out, ln1_g, ln1_b, ln2_g, ln2_b, router_w, router_bias, exp_w1, exp_w3, exp_w2):
    for l in range(DEPTH):
        p = jnp.einsum('btd,dc->btc', x, w_in[l])
        p_rw, p_att, p_lru = jnp.split(p, [RW_COLS, RW_COLS + ATT_COLS], axis=-1)
        y_rw = _rwkv7(p_rw, rw_mu[l], rw_w0[l], rw_w_up[l], rw_a0[l], rw_a_up[l], rw_g_up[l],
                      rw_k_k[l], rw_k_a[l], rw_r_k[l], rw_lnx_g[l], rw_lnx_b[l])
        y_att = _window_attention(p_att, att_sink[l])
        y_lru = _rglru(p_lru, lru_conv_w[l], lru_conv_b[l], lru_wa[l], lru_ba[l], lru_wx[l], lru_bx[l], lru_lambda[l])
        y = jnp.concatenate([y_rw, y_att, y_lru], axis=-1)
        h = jnp.einsum('btc,cd->btd', y, w_out[l])
        x = _layer_norm(ALPHA * x + h, ln1_g[l], ln1_b[l])
        f = _moe(x, router_w, router_bias, exp_w1[l], exp_w3[l], exp_w2[l])
        x = _layer_norm(ALPHA * x + f, ln2_g[l], ln2_b[l])
    return x


def setup_inputs(seed: int = 0) -> dict:
    key = jax.random.key(seed)
    k = jax.random.split(key, 32)

    def nrm(i, shape, scale):
        return scale * jax.random.normal(k[i], shape, jnp.float32)

    def uni(i, shape, lo, hi):
        return jax.random.uniform(k[i], shape, jnp.float32, lo, hi)

    L = DEPTH
    a_target = uni(21, (L, 2, LRU_WIDTH), 0.9, 0.999)
    sig_lam = a_target ** (1.0 / LRU_C)
    lru_lambda = jnp.log(sig_lam) - jnp.log1p(-sig_lam)
    return {
        'x_prompt': nrm(0, (BATCH, SEQ, D_MODEL), 1.0),
        'x_sample': nrm(1, (DEC_BATCH, DEC_SEQ, D_MODEL), 1.0),
        'w_in': nrm(2, (L, D_MODEL, IN_WIDTH), D_MODEL ** -0.5),
        'rw_mu': uni(3, (L, RW_COLS), 0.0, 0.5),
        'rw_w0': uni(4, (L, 2, RW_WIDTH), -6.0, -1.0),
        'rw_w_up': nrm(5, (L, 2, RW_DECAY_RANK, RW_WIDTH), 0.5 * RW_DECAY_RANK ** -0.5),
        'rw_a0': nrm(6, (L, 2, RW_WIDTH), 0.1),
        'rw_a_up': nrm(7, (L, 2, RW_ICLR_RANK, RW_WIDTH), RW_ICLR_RANK ** -0.5),
        'rw_g_up': nrm(8, (L, RW_GATE_RANK, RW_WIDTH), RW_GATE_RANK ** -0.5),
        'rw_k_k': 0.85 + nrm(9, (L, RW_WIDTH), 0.05),
        'rw_k_a': 1.0 + nrm(10, (L, RW_WIDTH), 0.05),
        'rw_r_k': nrm(11, (L, RW_HEADS, HEAD_DIM), 0.1),
        'rw_lnx_g': 1.0 + nrm(12, (L, RW_WIDTH), 0.05),
        'rw_lnx_b': nrm(13, (L, RW_WIDTH), 0.02),
        'att_sink': nrm(14, (L, ATT_Q_HEADS), 1.0),
        'lru_conv_w': nrm(15, (L, CONV_WIDTH, LRU_WIDTH), 0.5),
        'lru_conv_b': nrm(16, (L, LRU_WIDTH), 0.02),
        'lru_wa': nrm(17, (L, 2, LRU_BLOCKS, LRU_BLOCK_DIM, LRU_BLOCK_DIM), LRU_BLOCK_DIM ** -0.5),
        'lru_ba': nrm(18, (L, 2, LRU_WIDTH), 0.02),
        'lru_wx': nrm(19, (L, 2, LRU_BLOCKS, LRU_BLOCK_DIM, LRU_BLOCK_DIM), LRU_BLOCK_DIM ** -0.5),
        'lru_bx': nrm(20, (L, 2, LRU_WIDTH), 0.02),
        'lru_lambda': lru_lambda,
        'w_out': nrm(22, (L, MIX_WIDTH, D_MODEL), BETA * MIX_WIDTH ** -0.5),
        'ln1_g': 1.0 + nrm(23, (L, D_MODEL), 0.05),
        'ln1_b': nrm(24, (L, D_MODEL), 0.02),
        'ln2_g': 1.0 + nrm(25, (L, D_MODEL), 0.05),
        'ln2_b': nrm(26, (L, D_MODEL), 0.02),
        'router_w': nrm(27, (D_MODEL, N_EXPERTS), D_MODEL ** -0.5),
        'router_bias': nrm(28, (N_EXPERTS,), 0.01),
        'exp_w1': nrm(29, (L, N_EXPERTS, D_MODEL, D_FF_EXPERT), D_MODEL ** -0.5),
        'exp_w3': nrm(30, (L, N_EXPERTS, D_MODEL, D_FF_EXPERT), D_MODEL ** -0.5),
        'exp_w2': nrm(31, (L, N_EXPERTS, D_FF_EXPERT, D_MODEL), BETA * D_FF_EXPERT ** -0.5),
    }


def reference(x_prompt, x_sample, w_in, rw_mu, rw_w0, rw_w_up, rw_a0, rw_a_up, rw_g_up, rw_k_k, rw_k_a,
              rw_r_k, rw_lnx_g, rw_lnx_b, att_sink, lru_conv_w, lru_conv_b, lru_wa, lru_ba, lru_wx, lru_bx,
              lru_lambda, w_out, ln1_g, ln1_b, ln2_g, ln2_b, router_w, router_bias, exp_w1, exp_w3, exp_w2):
    y_prompt = _trunk(x_prompt, w_in, rw_mu, rw_w0, rw_w_up, rw_a0, rw_a_up, rw_g_up, rw_k_k, rw_k_a,
                      rw_r_k, rw_lnx_g, rw_lnx_b, att_sink, lru_conv_w, lru_conv_b, lru_wa, lru_ba, lru_wx,
                      lru_bx, lru_lambda, w_out, ln1_g, ln1_b, ln2_g, ln2_b, router_w, router_bias,
                      exp_w1, exp_w3, exp_w2)
    y_sample = _trunk(x_sample, w_in, rw_mu, rw_w0, rw_w_up, rw_a0, rw_a_up, rw_g_up, rw_k_k, rw_k_a,
                      rw_r_k, rw_lnx_g, rw_lnx_b, att_sink, lru_conv_w, lru_conv_b, lru_wa, lru_ba, lru_wx,
                      lru_bx, lru_lambda, w_out, ln1_g, ln1_b, ln2_g, ln2_b, router_w, router_bias,
                      exp_w1, exp_w3, exp_w2)
    return (y_prompt, y_sample)
```

```python
import numpy as np
import concourse.bass as bass
import concourse.mybir as mybir
from concourse.bass_utils import run_bass_kernel_spmd

F32 = mybir.dt.float32
BF16 = mybir.dt.bfloat16
AF = mybir.ActivationFunctionType
ALU = mybir.AluOpType
AX = mybir.AxisListType

D = 2048
DEPTH = 2
RW_W = 768
RW_COLS = 2560
ATT_W = 768
ATT_COLS = 1280
LRU_W = 512
LRU_COLS = 1024
IN_W = 4864
NE = 16
DFF = 1024
ALPHA = (2.0 * DEPTH) ** 0.25
LN_EPS = 1e-5
RW_GN_EPS = 64e-5
CH = 64
WSHAPES = [('w_in', (2, 2048, 4864)), ('rw_mu', (2, 2560)), ('rw_w0', (2, 2, 768)), ('rw_w_up', (2, 2, 64, 768)),
           ('rw_a0', (2, 2, 768)), ('rw_a_up', (2, 2, 64, 768)), ('rw_g_up', (2, 128, 768)), ('rw_k_k', (2, 768)),
           ('rw_k_a', (2, 768)), ('rw_r_k', (2, 12, 64)), ('rw_lnx_g', (2, 768)), ('rw_lnx_b', (2, 768)),
           ('att_sink', (2, 12)), ('lru_conv_w', (2, 4, 512)), ('lru_conv_b', (2, 512)), ('lru_wa', (2, 2, 8, 64, 64)),
           ('lru_ba', (2, 2, 512)), ('lru_wx', (2, 2, 8, 64, 64)), ('lru_bx', (2, 2, 512)), ('lru_lambda', (2, 2, 512)),
           ('w_out', (2, 2048, 2048)), ('ln1_g', (2, 2048)), ('ln1_b', (2, 2048)), ('ln2_g', (2, 2048)), ('ln2_b', (2, 2048)),
           ('router_w', (2048, 16)), ('router_bias', (16,)), ('exp_w1', (2, 16, 2048, 1024)), ('exp_w3', (2, 16, 2048, 1024)),
           ('exp_w2', (2, 16, 1024, 2048))]


class Ctx:
    NDS = 8

    def __init__(self, nc):
        self.nc = nc
        self.eng = {'pe': nc.tensor, 'dve': nc.vector, 'act': nc.scalar, 'pool': nc.gpsimd, 'sp': nc.sync}
        self.sem = {e: nc.alloc_semaphore("s_" + e) for e in ['pe', 'dve', 'act', 'pool']}
        self.cnt = {e: 0 for e in self.sem}
        self.seen = {e: {} for e in self.eng}
        self.lastw = {}
        self.readers = {}
        self.dsems = {q: [nc.alloc_semaphore("d_%s%d" % (q, i)) for i in range(self.NDS)] for q in ['sp', 'act', 'pool']}
        self.dval = {}
        self.semobj = {}
        for q in self.dsems:
            for s in self.dsems[q]:
                self.dval[id(s)] = 0
                self.semobj[id(s)] = s
        for e in self.sem:
            self.semobj[id(self.sem[e])] = self.sem[e]
        self.drr = {q: 0 for q in self.dsems}
        self.semowner = {id(self.sem[e]): e for e in self.sem}

    def _wait(self, e, tok):
        sid, val = tok
        if self.semowner.get(sid) == e and e == 'pe':
            return
        if self.seen[e].get(sid, 0) >= val:
            return
        self.eng[e].wait_ge(self.semobj[sid], val)
        self.seen[e][sid] = val

    def _deps(self, e, reads, writes):
        for k in reads:
            if k in self.lastw:
                self._wait(e, self.lastw[k])
        for k in writes:
            if k in self.lastw:
                self._wait(e, self.lastw[k])
            for sid, val in self.readers.get(k, {}).items():
                self._wait(e, (sid, val))

    def _record(self, tok, reads, writes):
        for k in writes:
            self.lastw[k] = tok
            self.readers[k] = {}
        for k in reads:
            r = self.readers.setdefault(k, {})
            if r.get(tok[0], 0) < tok[1]:
                r[tok[0]] = tok[1]

    def op(self, e, reads, writes, fn):
        self._deps(e, reads, writes)
        inst = fn(self.eng[e])
        self.cnt[e] += 1
        inst.then_inc(self.sem[e], 1)
        tok = (id(self.sem[e]), self.cnt[e])
        self.seen[e][tok[0]] = max(self.seen[e].get(tok[0], 0), 0)
        self._record(tok, reads, writes)

    def dma(self, q, out, in_, reads, writes, **kw):
        self._deps(q, reads, writes)
        s = self.dsems[q][self.drr[q] % self.NDS]
        self.drr[q] += 1
        sid = id(s)
        if self.dval[sid] > 0:
            self._wait(q, (sid, self.dval[sid]))
        inst = self.eng[q].dma_start(out=out, in_=in_, **kw)
        self.dval[sid] += 16
        inst.then_inc(s, 16)
        tok = (sid, self.dval[sid])
        self._record(tok, reads, writes)

    def barrier(self):
        toks = [(id(self.sem[e]), self.cnt[e]) for e in self.sem if self.cnt[e] > 0]
        for q in self.dsems:
            for s in self.dsems[q]:
                if self.dval[id(s)] > 0:
                    toks.append((id(s), self.dval[id(s)]))
        for e in self.eng:
            for t in toks:
                if self.semowner.get(t[0]) == e:
                    continue
                self._wait(e, t)
        self.lastw = {}
        self.readers = {}

    def finish(self):
        toks = []
        for q in self.dsems:
            for s in self.dsems[q]:
                if self.dval[id(s)] > 0:
                    toks.append((id(s), self.dval[id(s)]))
        for t in toks:
            self._wait('sp', t)


class Pool_:
    uid = 0

    def __init__(self, nc):
        self.nc = nc
        self.stack = []

    def tile(self, name, shape, dt):
        Pool_.uid += 1
        g = self.nc.sbuf_tensor("%s_u%d" % (name, Pool_.uid), shape, dt)
        t = g.__enter__()
        self.stack.append(g)
        return t

    def release(self):
        while self.stack:
            g = self.stack.pop()
            g.__exit__(None, None, None)


def build(SEG, dbg=None):
    T = 2 * SEG
    NT = T // 128
    nc = bass.Bass("TRN2", target_bir_lowering=False)

    def din(name, shape):
        return nc.dram_tensor(name, shape, F32, kind="ExternalInput").ap()

    x_in = din("x", [T, D])
    link = din("link", [128, 1])
    ident_d = din("ident", [128, 128])
    abias_d = din("abias", [128, 12, 384])
    blk_d = din("blk", [128, 128])
    m4_d = din("m4", [128, 2, 256])
    mT_d = din("mT", [128, 2, 64])
    idh_d = din("idh", [128, 64])
    class LazyW(dict):
        def __missing__(self, name):
            self[name] = din(name, list(dict(WSHAPES)[name]))
            return self[name]
    W = LazyW()
    w_in = W['w_in']
    y_out = nc.dram_tensor("y", [T, D], F32, kind="ExternalOutput").ap()
    skind = "ExternalOutput" if dbg else "Internal"
    pT = nc.dram_tensor("pT", [IN_W, T], F32, kind=skind).ap()
    yT = nc.dram_tensor("yT", [D, T], BF16, kind=skind).ap()

    cx = Ctx(nc)
    sb = Pool_(nc)
    ps = [nc.alloc_psum_tensor("ps%d" % i, [128, 512], F32) for i in range(8)]

    ident = nc.alloc_sbuf_tensor("ident_sb", [128, 128], F32)
    cx.dma('sp', ident[:], ident_d[:, :], [], ['ident'])

    def phase_inproj(l, xsrc, pdst):
        TS = min(2048, T)
        xT = sb.tile("xT", [128, 16, TS], BF16)
        xs = [sb.tile("xs%d" % i, [128, D], F32) for i in range(2)]
        wg = [sb.tile("wg%d" % i, [128, 16, 512], BF16) for i in range(2)]
        po = [sb.tile("po%d" % i, [128, 512], F32) for i in range(2)]
        wv = w_in[l].rearrange("(kc p) f -> p kc f", p=128)
        n = 0
        for ts in range(T // TS):
            for j in range(TS // 128):
                t0 = ts * TS + j * 128
                b = n % 2
                n += 1
                cx.dma('sp', xs[b][:], xsrc[t0:t0 + 128, :], [], ['xs%d' % b])
                for g4 in range(4):
                    pk = 'ps%d' % (g4 % 2)
                    for q in range(4):
                        kc = g4 * 4 + q
                        cx.op('pe', ['xs%d' % b, 'ident'], [pk],
                              lambda e, kc=kc, q=q, g4=g4: e.transpose(ps[g4 % 2][:, q * 128:(q + 1) * 128], xs[b][:, kc * 128:(kc + 1) * 128], ident[:]))
                    src = ps[g4 % 2][:].rearrange("p (q t) -> p q t", q=4)
                    dst = xT[:, g4 * 4:(g4 + 1) * 4, j * 128:(j + 1) * 128]
                    if g4 % 2 == 0:
                        cx.op('dve', [pk], ['xT'], lambda e, src=src, dst=dst: e.tensor_copy(dst, src))
                    else:
                        cx.op('act', [pk], ['xT'], lambda e, src=src, dst=dst: e.activation(dst, src, AF.Copy))
            ngrp = (IN_W + 511) // 512
            m = 0
            for g in range(ngrp):
                f0 = g * 512
                fw = min(512, IN_W - f0)
                b = g % 2
                cx.dma('pool', wg[b][:, :, 0:fw], wv[:, :, f0:f0 + fw], [], ['wg%d' % b])
                for fi in range(fw // 128):
                    for tc in range(TS // 512):
                        pk = 'ps%d' % (2 + m % 4)
                        pst = ps[2 + m % 4]
                        for kc in range(16):
                            cx.op('pe', ['wg%d' % b, 'xT'], [pk],
                                  lambda e, kc=kc, fi=fi, tc=tc, pst=pst: e.matmul(pst[:], wg[b][:, kc, fi * 128:(fi + 1) * 128],
                                                                                   xT[:, kc, tc * 512:(tc + 1) * 512],
                                                                                   start=(kc == 0), stop=(kc == 15)))
                        ob = m % 2
                        if m % 2 == 0:
                            cx.op('dve', [pk], ['po%d' % ob], lambda e, pst=pst, ob=ob: e.tensor_copy(po[ob][:], pst[:]))
                        else:
                            cx.op('act', [pk], ['po%d' % ob], lambda e, pst=pst, ob=ob: e.activation(po[ob][:], pst[:], AF.Copy))
                        tk0 = ts * TS + tc * 512
                        cx.dma('sp', pdst[f0 + fi * 128:f0 + (fi + 1) * 128, tk0:tk0 + 512], po[ob][:], ['po%d' % ob], ['pT'])
                        m += 1
        cx.barrier()
        sb.release()

    linkt = nc.alloc_sbuf_tensor("link_sb", [128, 1], F32)
    cx.dma('sp', linkt[:], link[:, :], [], ['link'])

    def col(v):
        return v.rearrange("(p o) -> p o", o=1)

    def phase_lru(l):
        TB = min(2048, SEG)
        nb = T // TB
        LB = RW_COLS + ATT_COLS
        xc_all = sb.tile("xc_all", [128, T], F32)
        hf_all = sb.tile("hf_all", [128, T], F32)
        xh = sb.tile("xh", [128, TB + 4], F32)
        xcb = sb.tile("xcb", [128, TB], BF16)
        ta = sb.tile("ta", [128, TB], F32)
        tb_ = sb.tile("tb", [128, TB], F32)
        tc_ = sb.tile("tc", [128, TB], F32)
        td = sb.tile("td", [128, TB], F32)
        gt = sb.tile("gt", [128, TB], F32)
        ob = sb.tile("ob", [128, TB], BF16)
        cw = sb.tile("cw", [128, 4], F32)
        cb = sb.tile("cb", [128, 1], F32)
        prm = sb.tile("prm", [128, 8], F32)
        carry = sb.tile("carry", [128, 2], F32)
        wst = sb.tile("wst", [128, 4, 128], F32)
        wbd = sb.tile("wbd", [128, 4, 128], BF16)
        for ci in range(4):
            c0 = ci * 128
            with nc.allow_non_contiguous_dma(reason="tiny param loads"):
                cx.dma('sp', cw[:], W['lru_conv_w'][l][:, c0:c0 + 128].rearrange("j c -> c j"), [], ['cw'])
            cx.dma('sp', cb[:], col(W['lru_conv_b'][l][c0:c0 + 128]), [], ['cb'])
            for d in range(2):
                cx.dma('sp', prm[:, d:d + 1], col(W['lru_ba'][l, d][c0:c0 + 128]), [], ['prm'])
                cx.dma('sp', prm[:, 2 + d:3 + d], col(W['lru_bx'][l, d][c0:c0 + 128]), [], ['prm'])
                cx.dma('sp', prm[:, 4 + d:5 + d], col(W['lru_lambda'][l, d][c0:c0 + 128]), [], ['prm'])
            cx.op('act', ['prm'], ['prm'], lambda e: e.activation(prm[:, 6:8], prm[:, 4:6], AF.Exp, scale=-1.0))
            cx.op('act', ['prm'], ['prm'], lambda e: e.activation(prm[:, 6:8], prm[:, 6:8], AF.Ln, bias=1.0))
            cx.op('dve', ['prm'], ['prm'], lambda e: e.tensor_scalar(prm[:, 6:8], prm[:, 6:8], -8.0, None, ALU.mult))
            cx.op('pool', [], ['wst'], lambda e: e.memset(wst[:], 0.0))
            for wi, (nm, d) in enumerate([('lru_wa', 0), ('lru_wa', 1), ('lru_wx', 0), ('lru_wx', 1)]):
                for hb in range(2):
                    cx.dma('sp', wst[hb * 64:(hb + 1) * 64, wi, hb * 64:(hb + 1) * 64], W[nm][l, d, 2 * ci + hb], [], ['wst'])
            cx.op('dve', ['wst'], ['wbd'], lambda e: e.tensor_copy(wbd[:], wst[:]))

            def gates(d, t0):
                CK = min(512, TB)
                for tcn in range(TB // CK):
                    sl = slice(tcn * CK, (tcn + 1) * CK)
                    cx.op('pe', ['wbd', 'xcb'], ['ps0'], lambda e: e.matmul(ps[0][:, 0:CK], wbd[:, d, :], xcb[:, sl], start=True, stop=True))
                    cx.op('pe', ['wbd', 'xcb'], ['ps1'], lambda e: e.matmul(ps[1][:, 0:CK], wbd[:, 2 + d, :], xcb[:, sl], start=True, stop=True))
                    cx.op('act', ['ps0', 'prm'], ['ta'], lambda e: e.activation(ta[:, sl], ps[0][:, 0:CK], AF.Sigmoid, bias=prm[:, d:d + 1]))
                    cx.op('act', ['ps1', 'prm'], ['tb'], lambda e: e.activation(tb_[:, sl], ps[1][:, 0:CK], AF.Sigmoid, bias=prm[:, 2 + d:3 + d]))
                cx.op('act', ['ta', 'prm'], ['ta'], lambda e: e.activation(ta[:], ta[:], AF.Exp, scale=prm[:, 6 + d:7 + d]))
                cx.op('pool', ['ta'], ['tc'], lambda e: e.tensor_tensor(tc_[:], ta[:], ta[:], ALU.mult))
                cx.op('dve', ['tc'], ['tc'], lambda e: e.tensor_scalar(tc_[:], tc_[:], -1.0, 1.0, ALU.mult, ALU.add))
                cx.op('act', ['tc'], ['tc'], lambda e: e.activation(tc_[:], tc_[:], AF.Sqrt))
                cx.op('dve', ['tc', 'tb'], ['tb'], lambda e: e.tensor_tensor(tb_[:], tb_[:], tc_[:], ALU.mult))
                cx.op('dve', ['tb', 'xc_all'], ['tb'], lambda e: e.tensor_tensor(tb_[:], tb_[:], xc_all[:, t0:t0 + TB], ALU.mult))

            for bi in range(nb):
                t0 = bi * TB
                lo = t0 - 2
                hi = t0 + TB + 1
                if bi == 0:
                    cx.op('pool', [], ['xh'], lambda e: e.memset(xh[:, 0:2], 0.0))
                    lo = t0
                if bi == nb - 1:
                    cx.op('pool', [], ['xh'], lambda e: e.memset(xh[:, TB + 2:TB + 3], 0.0))
                    hi = t0 + TB
                cx.dma('sp', xh[:, 2 + (lo - t0):2 + (hi - t0)], pT[LB + c0:LB + c0 + 128, lo:hi], ['pT'], ['xh'])
                if t0 == SEG:
                    cx.op('dve', ['xh', 'link'], ['xh'], lambda e: e.tensor_scalar(xh[:, 0:2], xh[:, 0:2], linkt[:, 0:1], None, ALU.mult))
                if t0 + TB == SEG:
                    cx.op('dve', ['xh', 'link'], ['xh'], lambda e: e.tensor_scalar(xh[:, TB + 2:TB + 3], xh[:, TB + 2:TB + 3], linkt[:, 0:1], None, ALU.mult))
                xcs = xc_all[:, t0:t0 + TB]
                cx.op('dve', ['xh', 'cw', 'cb'], ['xc_all'], lambda e: e.tensor_scalar(xcs, xh[:, 0:TB], cw[:, 0:1], cb[:, 0:1], ALU.mult, ALU.add))
                for j in range(1, 4):
                    cx.op('dve', ['xh', 'cw', 'xc_all'], ['xc_all'],
                          lambda e, j=j: e.scalar_tensor_tensor(xcs, xh[:, j:j + TB], cw[:, j:j + 1], xcs, ALU.mult, ALU.add))
                cx.op('act', ['xc_all'], ['xcb'], lambda e: e.activation(xcb[:], xcs, AF.Copy))
                gates(0, t0)
                if bi == 0:
                    init = 0.0
                elif t0 == SEG:
                    cx.op('dve', ['hf_all', 'link'], ['carry'], lambda e: e.tensor_scalar(carry[:, 0:1], hf_all[:, t0 - 1:t0], linkt[:, 0:1], None, ALU.mult))
                    init = carry[:, 0:1]
                else:
                    init = hf_all[:, t0 - 1:t0]
                cx.op('dve', ['ta', 'tb', 'carry', 'hf_all'], ['hf_all'],
                      lambda e: e.tensor_tensor_scan(hf_all[:, t0:t0 + TB], ta[:], tb_[:], init, ALU.mult, ALU.add))
            for bi in range(nb - 1, -1, -1):
                t0 = bi * TB
                xcs = xc_all[:, t0:t0 + TB]
                cx.op('act', ['xc_all'], ['xcb'], lambda e: e.activation(xcb[:], xcs, AF.Copy))
                gates(1, t0)
                if bi == nb - 1:
                    init = 0.0
                elif t0 + TB == SEG:
                    cx.op('dve', ['carry', 'link'], ['carry'], lambda e: e.tensor_scalar(carry[:, 1:2], carry[:, 1:2], linkt[:, 0:1], None, ALU.mult))
                    init = carry[:, 1:2]
                else:
                    init = carry[:, 1:2]
                cx.op('dve', ['ta', 'tb', 'carry'], ['td'],
                      lambda e: e.tensor_tensor_scan(td[:, ::-1], ta[:, ::-1], tb_[:, ::-1], init, ALU.mult, ALU.add))
                cx.op('pool', ['td'], ['carry'], lambda e: e.tensor_copy(carry[:, 1:2], td[:, 0:1]))
                cx.op('dve', ['td', 'hf_all'], ['td'], lambda e: e.tensor_tensor(td[:], td[:], hf_all[:, t0:t0 + TB], ALU.add))
                cx.dma('sp', gt[:], pT[LB + LRU_W + c0:LB + LRU_W + c0 + 128, t0:t0 + TB], ['pT'], ['gt'])
                cx.op('pool', ['gt'], ['tc'], lambda e: e.tensor_tensor(tc_[:], gt[:], gt[:], ALU.mult))
                cx.op('dve', ['tc'], ['tc'], lambda e: e.tensor_scalar(tc_[:], tc_[:], 0.044715, 1.0, ALU.mult, ALU.add))
                cx.op('pool', ['tc', 'gt'], ['tc'], lambda e: e.tensor_tensor(tc_[:], tc_[:], gt[:], ALU.mult))
                cx.op('act', ['tc'], ['tc'], lambda e: e.activation(tc_[:], tc_[:], AF.Sigmoid, scale=1.5957691216057308))
                cx.op('pool', ['tc', 'gt'], ['tc'], lambda e: e.tensor_tensor(tc_[:], tc_[:], gt[:], ALU.mult))
                cx.op('dve', ['tc', 'td'], ['ob'], lambda e: e.tensor_tensor(ob[:], tc_[:], td[:], ALU.mult))
                cx.dma('sp', yT[RW_W + ATT_W + c0:RW_W + ATT_W + c0 + 128, t0:t0 + TB], ob[:], ['ob'], ['yT'])
        cx.barrier()
        sb.release()

    identb = nc.alloc_sbuf_tensor("identb_sb", [128, 128], BF16)
    cx.op('dve', ['ident'], ['identb'], lambda e: e.tensor_copy(identb[:], ident[:]))
    psb = [p[:].bitcast(BF16) for p in ps]

    def phase_att(l):
        NBS = SEG // 128
        AB = RW_COLS
        ab = sb.tile("abias", [128, 12, 384], F32)
        cx.dma('sp', ab[:], abias_d[:, :, :], [], ['abias'])
        sinkt = sb.tile("sinkt", [128, 12], F32)
        cx.dma('sp', sinkt[:], W['att_sink'][l].partition_broadcast(128), [], ['sinkt'])
        cutb = sb.tile("cutb", [128, 1], F32)
        cx.op('dve', ['link'], ['cutb'], lambda e: e.tensor_scalar(cutb[:], linkt[:], -1.0, 30000.0, ALU.add, ALU.mult))
        vtm = sb.tile("vtm", [128, NT, 256], BF16)
        vld = [sb.tile("vld%d" % i, [128, 2, 128], BF16) for i in range(2)]
        for nb in range(NT):
            t0 = nb * 128
            b = nb % 2
            cx.dma('pool', vld[b][:], pT[AB + 1024:AB + 1280, t0:t0 + 128].rearrange("(a p) t -> p a t", p=128), ['pT'], ['vld%d' % b])
            pk = 'ps%d' % b
            for a in range(2):
                cx.op('pe', ['vld%d' % b, 'identb'], [pk], lambda e: e.transpose(psb[b][:, a * 128:(a + 1) * 128], vld[b][:, a, :], identb[:]))
            if b == 0:
                cx.op('dve', [pk], ['vtm'], lambda e: e.tensor_copy(vtm[:, nb, :], psb[b][:, 0:256]))
            else:
                cx.op('act', [pk], ['vtm'], lambda e: e.activation(vtm[:, nb, :], psb[b][:, 0:256], AF.Copy))
        qb = [sb.tile("qb%d" % i, [128, 6, 128], BF16) for i in range(2)]
        kd = [sb.tile("kd%d" % i, [128, 4, 384], BF16) for i in range(2)]
        sc = [sb.tile("sc%d" % i, [128, 384], F32) for i in range(2)]
        eb = [sb.tile("eb%d" % i, [128, 384], BF16) for i in range(2)]
        eT = [sb.tile("eT%d" % i, [128, 3, 128], BF16) for i in range(2)]
        st = [sb.tile("st%d" % i, [128, 8], F32) for i in range(2)]
        oall = [sb.tile("oall%d" % i, [128, 768], BF16) for i in range(2)]
        ot = [sb.tile("ot%d" % i, [128, 6, 128], BF16) for i in range(2)]
        hn = 0
        for nb in range(NT):
            t0 = nb * 128
            seg, j = nb // NBS, nb % NBS
            left = 'y' if j > 0 else ('l' if seg == 1 else 'n')
            right = 'y' if j < NBS - 1 else ('l' if seg == 0 else 'n')
            klo = t0 - 128 if left != 'n' else t0
            khi = t0 + 256 if right != 'n' else t0 + 128
            ncol = khi - klo
            off = klo - (t0 - 128)
            bb = nb % 2
            cx.dma('pool', qb[bb][:], pT[AB:AB + 768, t0:t0 + 128].rearrange("(a p) t -> p a t", p=128), ['pT'], ['qb%d' % bb])
            for hb in range(2):
                cx.dma('pool', kd[bb][hb * 64:(hb + 1) * 64, :, 0:ncol],
                       pT[AB + 768:AB + 1024, klo:khi].rearrange("(h d) t -> d h t", d=64), ['pT'], ['kd%d' % bb])
            for qh in range(12):
                kvh = qh // 3
                H = (qh % 2) * 64
                u = hn % 2
                hn += 1
                pss, pst, pso = 'ps%d' % (2 + u), 'ps%d' % (4 + u), 'ps%d' % (6 + u)
                S = ['sc%d' % u]
                cx.op('pe', ['qb%d' % bb, 'kd%d' % bb], [pss],
                      lambda e: e.matmul(ps[2 + u][:, 0:ncol], qb[bb][H:H + 64, qh // 2, :], kd[bb][H:H + 64, kvh, 0:ncol], start=True, stop=True))
                cx.op('dve', [pss, 'abias'], S,
                      lambda e: e.scalar_tensor_tensor(sc[u][:, 0:ncol], ps[2 + u][:, 0:ncol], 0.125, ab[:, qh, off:off + ncol], ALU.mult, ALU.add))
                if left == 'l':
                    cx.op('dve', S + ['cutb'], S, lambda e: e.tensor_scalar(sc[u][:, 0:128], sc[u][:, 0:128], cutb[:, 0:1], None, ALU.add))
                if right == 'l':
                    cx.op('dve', S + ['cutb'], S, lambda e: e.tensor_scalar(sc[u][:, ncol - 128:ncol], sc[u][:, ncol - 128:ncol], cutb[:, 0:1], None, ALU.add))
                SK = ['st%d' % u]
                cx.op('dve', S, SK, lambda e: e.tensor_reduce(st[u][:, 0:1], sc[u][:, 0:ncol], AX.X, ALU.max))
                cx.op('dve', SK + ['sinkt'], SK, lambda e: e.tensor_scalar(st[u][:, 1:2], st[u][:, 0:1], sinkt[:, qh:qh + 1], -1.0, ALU.max, ALU.mult))
                cx.op('act', S + SK, ['eb%d' % u] + SK,
                      lambda e: e.activation(eb[u][:, 0:ncol], sc[u][:, 0:ncol], AF.Exp, bias=st[u][:, 1:2], accum_out=st[u][:, 2:3]))
                cx.op('act', SK + ['sinkt'], SK, lambda e: e.activation(st[u][:, 3:4], st[u][:, 1:2], AF.Exp, bias=sinkt[:, qh:qh + 1]))
                cx.op('dve', SK, SK, lambda e: e.tensor_tensor(st[u][:, 4:5], st[u][:, 2:3], st[u][:, 3:4], ALU.add))
                cx.op('dve', SK, SK, lambda e: e.reciprocal(st[u][:, 5:6], st[u][:, 4:5]))
                nkb = ncol // 128
                for kb in range(nkb):
                    cx.op('pe', ['eb%d' % u, 'identb'], [pst],
                          lambda e: e.transpose(psb[4 + u][:, kb * 128:(kb + 1) * 128], eb[u][:, kb * 128:(kb + 1) * 128], identb[:]))
                cx.op('act', [pst], ['eT%d' % u],
                      lambda e: e.activation(eT[u][:, 0:nkb, :], psb[4 + u][:, 0:nkb * 128].rearrange("p (k t) -> p k t", k=nkb), AF.Copy))
                kb0 = klo // 128
                for kb in range(nkb):
                    cx.op('pe', ['eT%d' % u, 'vtm'], [pso],
                          lambda e: e.matmul(ps[6 + u][:, 0:64], eT[u][:, kb, :], vtm[:, kb0 + kb, kvh * 64:(kvh + 1) * 64],
                                             start=(kb == 0), stop=(kb == nkb - 1)))
                cx.op('dve', [pso] + SK, ['oall%d' % bb],
                      lambda e: e.tensor_scalar(oall[bb][:, qh * 64:(qh + 1) * 64], ps[6 + u][:, 0:64], st[u][:, 5:6], None, ALU.mult))
            for a in range(6):
                cx.op('pe', ['oall%d' % bb, 'identb'], ['ps%d' % bb],
                      lambda e: e.transpose(psb[bb][:, a * 128:(a + 1) * 128], oall[bb][:, a * 128:(a + 1) * 128], identb[:]))
            cx.op('act', ['ps%d' % bb], ['ot%d' % bb],
                  lambda e: e.activation(ot[bb][:], psb[bb][:, 0:768].rearrange("p (a t) -> p a t", a=6), AF.Copy))
            cx.dma('sp', yT[RW_W:RW_W + ATT_W, t0:t0 + 128].rearrange("(a p) t -> p a t", p=128), ot[bb][:], ['ot%d' % bb], ['yT'])
        cx.barrier()
        sb.release()

    NCH = T // CH
    opT = nc.dram_tensor("rw_opT", [2, 4, RW_W, T], BF16, kind=skind).ap()
    vTs = nc.dram_tensor("rw_vT", [RW_W, T], BF16, kind=skind).ap()
    gams = nc.dram_tensor("rw_gam", [2, RW_W, NCH], F32, kind=skind).ap()
    bons = nc.dram_tensor("rw_bon", [RW_W, T], F32, kind=skind).ap()
    gTs = nc.dram_tensor("rw_gT", [RW_W, T], F32, kind=skind).ap()
    ysc = nc.dram_tensor("rw_ysc", [2, RW_W, T], F32, kind=skind).ap()
    blk = nc.alloc_sbuf_tensor("blk_sb", [128, 128], F32)
    cx.dma('sp', blk[:], blk_d[:, :], [], ['blk'])

    def phase_rwprep(l):
        TB = min(512, SEG)
        nbk = T // TB
        ncb = TB // CH
        P = [sb.tile("P%d" % i, [128, TB], F32) for i in range(20)]
        ph = [sb.tile("ph%d" % i, [128, TB + 2], F32) for i in range(2)]
        tq = [sb.tile("tq%d" % i, [128, TB], F32) for i in range(2)]
        mu = sb.tile("mu", [128, 20], F32)
        hmu = sb.tile("hmu", [128, 20], F32)
        omm = sb.tile("omm", [128, 20], F32)
        pr = sb.tile("pr", [128, 8, 6], F32)
        with nc.allow_non_contiguous_dma(reason="tiny param loads"):
            cx.dma('sp', mu[:], W['rw_mu'][l].rearrange("(a p) -> p a", p=128), [], ['mu'])
            for d in range(2):
                cx.dma('sp', pr[:, d, :], W['rw_w0'][l, d].rearrange("(a p) -> p a", p=128), [], ['pr'])
                cx.dma('sp', pr[:, 2 + d, :], W['rw_a0'][l, d].rearrange("(a p) -> p a", p=128), [], ['pr'])
            cx.dma('sp', pr[:, 4, :], W['rw_k_k'][l].rearrange("(a p) -> p a", p=128), [], ['pr'])
            cx.dma('sp', pr[:, 5, :], W['rw_k_a'][l].rearrange("(a p) -> p a", p=128), [], ['pr'])
            cx.dma('sp', pr[:, 7, :], W['rw_r_k'][l].rearrange("h k -> (h k)").rearrange("(a p) -> p a", p=128), [], ['pr'])
        cx.op('dve', ['mu'], ['hmu'], lambda e: e.tensor_scalar(hmu[:], mu[:], 0.5, None, ALU.mult))
        cx.op('dve', ['mu'], ['omm'], lambda e: e.tensor_scalar(omm[:], mu[:], -1.0, 1.0, ALU.mult, ALU.add))
        cx.op('dve', ['pr'], ['pr'], lambda e: e.tensor_scalar(pr[:, 6, :], pr[:, 5, :], -1.0, 1.0, ALU.mult, ALU.add))
        cx.op('dve', ['pr'], ['pr'], lambda e: e.tensor_scalar(pr[:, 7, :], pr[:, 7, :], 0.5, None, ALU.mult))
        wup = sb.tile("wup", [128, 2, RW_W], BF16)
        gup = sb.tile("gup", [128, RW_W], BF16)
        for d in range(2):
            cx.dma('pool', wup[0:64, d, :], W['rw_w_up'][l, d], [], ['wup'])
            cx.dma('pool', wup[64:128, d, :], W['rw_a_up'][l, d], [], ['wup'])
        cx.dma('pool', gup[:], W['rw_g_up'][l], [], ['gup'])
        mk = sb.tile("mk", [128, 2, TB], F32)
        cx.op('pool', [], ['mk'], lambda e: e.memset(mk[:], 1.0))
        cx.op('pool', [], ['mk'], lambda e: e.memset(mk[:, 0, :].rearrange("p (c j) -> p c j", j=CH)[:, :, 0:1], 0.0))
        cx.op('pool', [], ['mk'], lambda e: e.memset(mk[:, 1, :].rearrange("p (c j) -> p c j", j=CH)[:, :, CH - 1:CH], 0.0))
        twa = sb.tile("twa", [128, TB], BF16)
        sgd = sb.tile("sgd", [128, TB], BF16)
        kkf = sb.tile("kkf", [128, TB], F32)
        kk = sb.tile("kk", [128, TB], F32)
        t1 = sb.tile("t1", [128, TB], F32)
        t2 = sb.tile("t2", [128, TB], F32)
        t3 = sb.tile("t3", [128, TB], F32)
        ad = sb.tile("ad", [128, TB], F32)
        lw = sb.tile("lw", [128, TB], F32)
        ci = sb.tile("ci", [128, TB], F32)
        gi = sb.tile("gi", [128, TB], F32)
        ge = sb.tile("ge", [128, TB], F32)
        gv = sb.tile("gv", [128, TB], F32)
        km = sb.tile("km", [128, TB], F32)
        ks = sb.tile("ks", [128, TB], F32)
        gmt = sb.tile("gmt", [128, 2, ncb], F32)
        ob = [[sb.tile("ob%d_%d" % (d, q), [128, TB], BF16) for q in range(4)] for d in range(2)]
        vb = sb.tile("vb", [128, TB], BF16)
        gsb = sb.tile("gsb", [128, TB], F32)
        bsb = sb.tile("bsb", [128, TB], F32)
        n = 0
        for bk in range(nbk):
            t0 = bk * TB
            for i in range(20):
                b = n % 2
                n += 1
                H_ = 'ph%d' % b
                lo, hi = t0 - 1, t0 + TB + 1
                if t0 % SEG == 0 and t0 != SEG:
                    cx.op('pool', [], [H_], lambda e: e.memset(ph[b][:, 0:1], 0.0))
                    lo = t0
                if (t0 + TB) % SEG == 0 and t0 + TB != SEG:
                    cx.op('pool', [], [H_], lambda e: e.memset(ph[b][:, TB + 1:TB + 2], 0.0))
                    hi = t0 + TB
                cx.dma('sp', ph[b][:, 1 + (lo - t0):1 + (hi - t0)], pT[i * 128:(i + 1) * 128, lo:hi], ['pT'], [H_])
                if t0 == SEG:
                    cx.op('dve', [H_, 'link'], [H_], lambda e: e.tensor_scalar(ph[b][:, 0:1], ph[b][:, 0:1], linkt[:, 0:1], None, ALU.mult))
                if t0 + TB == SEG:
                    cx.op('dve', [H_, 'link'], [H_], lambda e: e.tensor_scalar(ph[b][:, TB + 1:TB + 2], ph[b][:, TB + 1:TB + 2], linkt[:, 0:1], None, ALU.mult))
                Q_ = 'tq%d' % b
                cx.op('pool', [H_], [Q_], lambda e: e.tensor_tensor(tq[b][:], ph[b][:, 0:TB], ph[b][:, 2:TB + 2], ALU.add))
                cx.op('act', [H_, 'omm'], ['P%d' % i], lambda e: e.activation(P[i][:], ph[b][:, 1:TB + 1], AF.Copy, scale=omm[:, i:i + 1]))
                cx.op('dve', [Q_, 'hmu', 'P%d' % i], ['P%d' % i],
                      lambda e: e.scalar_tensor_tensor(P[i][:], tq[b][:], hmu[:, i:i + 1], P[i][:], ALU.mult, ALU.add))
            cx.op('act', ['P18'], ['twa'], lambda e: e.activation(twa[0:64, :], P[18][0:64, :], AF.Tanh))
            cx.op('act', ['P18'], ['twa'], lambda e: e.activation(twa[64:128, :], P[18][64:128, :], AF.Copy))
            cx.op('act', ['P19'], ['sgd'], lambda e: e.activation(sgd[:], P[19][:], AF.Sigmoid))
            for ct in range(6):
                cs = slice(ct * 128, (ct + 1) * 128)
                rP, kP, vP = P[ct], P[6 + ct], P[12 + ct]
                rK, kK, vK = 'P%d' % ct, 'P%d' % (6 + ct), 'P%d' % (12 + ct)
                cx.op('pool', [kK, 'pr'], ['kkf'], lambda e: e.tensor_scalar(kkf[:], kP[:], pr[:, 4, ct:ct + 1], None, ALU.mult))
                cx.op('act', ['kkf'], ['t1'], lambda e: e.activation(t1[:], kkf[:], AF.Square))
                cx.op('pe', ['t1', 'blk'], ['ps0'], lambda e: e.matmul(ps[0][:, 0:TB], blk[:], t1[:], start=True, stop=True))
                cx.op('act', ['ps0'], ['t2'], lambda e: e.activation(t2[:], ps[0][:, 0:TB], AF.Sqrt))
                cx.op('dve', ['t2'], ['t2'], lambda e: e.tensor_scalar(t2[:], t2[:], 1e-12, None, ALU.max))
                cx.op('dve', ['t2'], ['t2'], lambda e: e.reciprocal(t2[:], t2[:]))
                cx.op('pool', ['kkf', 't2'], ['kk'], lambda e: e.tensor_tensor(kk[:], kkf[:], t2[:], ALU.mult))
                cx.op('pe', ['gup', 'sgd'], ['ps1'], lambda e: e.matmul(ps[1][:, 0:TB], gup[:, cs], sgd[:], start=True, stop=True))
                cx.op('act', ['ps1'], ['gsb'], lambda e: e.activation(gsb[:], ps[1][:, 0:TB], AF.Copy))
                cx.dma('sp', gTs[cs, t0:t0 + TB], gsb[:], ['gsb'], ['gTs'])
                for d in range(2):
                    cx.op('pe', ['wup', 'twa'], ['ps2'], lambda e: e.matmul(ps[2][:, 0:TB], wup[0:64, d, cs], twa[0:64, :], start=True, stop=True))
                    cx.op('pe', ['wup', 'twa'], ['ps3'], lambda e: e.matmul(ps[3][:, 0:TB], wup[64:128, d, cs], twa[64:128, :], start=True, stop=True))
                    cx.op('act', ['ps2', 'pr'], ['lw'], lambda e: e.activation(lw[:], ps[2][:, 0:TB], AF.Sigmoid, bias=pr[:, d, ct:ct + 1]))
                    cx.op('act', ['ps3', 'pr'], ['ad'], lambda e: e.activation(ad[:], ps[3][:, 0:TB], AF.Sigmoid, bias=pr[:, 2 + d, ct:ct + 1]))
                    cx.op('pool', ['lw'], ['lw'], lambda e: e.tensor_scalar(lw[:], lw[:], -0.6065306597126334, None, ALU.mult))
                    if d == 0:
                        cx.op('dve', ['lw', 'mk'], ['ci'], lambda e: e.tensor_tensor_scan(ci[:], mk[:, 0, :], lw[:], 0.0, ALU.mult, ALU.add))
                    else:
                        cx.op('dve', ['lw', 'mk'], ['ci'], lambda e: e.tensor_tensor_scan(ci[:, ::-1], mk[:, 1, ::-1], lw[:, ::-1], 0.0, ALU.mult, ALU.add))
                    cx.op('pool', ['ci', 'lw'], ['t3'], lambda e: e.tensor_tensor(t3[:], ci[:], lw[:], ALU.subtract))
                    cx.op('act', ['ci'], ['gi'], lambda e: e.activation(gi[:], ci[:], AF.Exp))
                    cx.op('act', ['t3'], ['ge'], lambda e: e.activation(ge[:], t3[:], AF.Exp))
                    cx.op('act', ['ci'], ['gv'], lambda e: e.activation(gv[:], ci[:], AF.Exp, scale=-1.0))
                    gsel = CH - 1 if d == 0 else 0
                    cx.op('pool', ['gi'], ['gmt'], lambda e: e.tensor_copy(gmt[:, d, :], gi[:].rearrange("p (c j) -> p c j", j=CH)[:, :, gsel]))
                    cx.dma('sp', gams[d, cs, t0 // CH:t0 // CH + ncb], gmt[:, d, :], ['gmt'], ['gams'])
                    cx.op('dve', ['ad', 'pr'], ['t1'], lambda e: e.tensor_scalar(t1[:], ad[:], pr[:, 5, ct:ct + 1], pr[:, 6, ct:ct + 1], ALU.mult, ALU.add))
                    cx.op('pool', ['t1', kK], ['km'], lambda e: e.tensor_tensor(km[:], t1[:], kP[:], ALU.mult))
                    if d == 0:
                        cx.op('pool', ['km'], ['ks'], lambda e: e.tensor_copy(ks[:], km[:]))
                    else:
                        cx.op('pool', ['km', 'ks'], ['ks'], lambda e: e.tensor_tensor(ks[:], ks[:], km[:], ALU.add))
                    O = ['ob%d_%d' % (d, q) for q in range(4)]
                    cx.op('dve', ['kk', 'ge'], [O[0]], lambda e: e.scalar_tensor_tensor(ob[d][0][:], kk[:], -1.0, ge[:], ALU.mult, ALU.mult))
                    cx.op('dve', [rK, 'gi'], [O[1]], lambda e: e.tensor_tensor(ob[d][1][:], rP[:], gi[:], ALU.mult))
                    cx.op('pool', ['kk', 'ad'], ['t2'], lambda e: e.tensor_tensor(t2[:], kk[:], ad[:], ALU.mult))
                    cx.op('dve', ['t2', 'gv'], [O[2]], lambda e: e.tensor_tensor(ob[d][2][:], t2[:], gv[:], ALU.mult))
                    cx.op('dve', ['km', 'gv'], [O[3]], lambda e: e.tensor_tensor(ob[d][3][:], km[:], gv[:], ALU.mult))
                    for q in range(4):
                        cx.dma('sp', opT[d, q, cs, t0:t0 + TB], ob[d][q][:], [O[q]], ['opT'])
                cx.op('dve', ['ks', 'pr', rK], ['t1'], lambda e: e.scalar_tensor_tensor(t1[:], ks[:], pr[:, 7, ct:ct + 1], rP[:], ALU.mult, ALU.mult))
                cx.op('pe', ['t1', 'blk'], ['ps4'], lambda e: e.matmul(ps[4][:, 0:TB], blk[:], t1[:], start=True, stop=True))
                cx.op('dve', ['ps4', vK], ['bsb'], lambda e: e.tensor_tensor(bsb[:], ps[4][:, 0:TB], vP[:], ALU.mult))
                cx.dma('sp', bons[cs, t0:t0 + TB], bsb[:], ['bsb'], ['bons'])
                cx.op('act', [vK], ['vb'], lambda e: e.activation(vb[:], vP[:], AF.Copy))
                cx.dma('sp', vTs[cs, t0:t0 + TB], vb[:], ['vb'], ['vTs'])
        cx.barrier()
        sb.release()

    def phase_rwscan(l):
        TBS = min(256, SEG)
        cpb = TBS // CH
        nblk = T // TBS
        NBS = SEG // TBS
        m4 = sb.tile("m4", [128, 2, 256], F32)
        cx.dma('sp', m4[:], m4_d[:, :, :], [], ['m4'])
        mT = sb.tile("mT", [128, 2, 64], F32)
        cx.dma('sp', mT[:], mT_d[:, :, :], [], ['mT'])
        idh = sb.tile("idh", [128, 64], F32)
        cx.dma('sp', idh[:], idh_d[:, :], [], ['idh'])
        units = [(i, d) for d in range(2) for i in range(6)]
        U_ = {}
        for (i, d) in units:
            u = "%d_%d" % (i, d)
            U_[(i, d)] = dict(
                AR=[sb.tile("AR" + u + "_%d" % z, [128, cpb, 128], BF16) for z in range(2)],
                BT=[sb.tile("BT" + u + "_%d" % z, [128, TBS], BF16) for z in range(2)],
                KT=[sb.tile("KT" + u + "_%d" % z, [128, TBS], BF16) for z in range(2)],
                VT=[sb.tile("VT" + u + "_%d" % z, [128, TBS], BF16) for z in range(2)],
                GM=[sb.tile("GM" + u + "_%d" % z, [128, cpb], F32) for z in range(2)],
                YB=[sb.tile("YB" + u + "_%d" % z, [128, TBS], F32) for z in range(2)],
                M4=sb.tile("M4" + u, [128, 256], BF16),
                AA=[sb.tile("AA" + u + "_%d" % z, [128, 128], BF16) for z in range(2)],
                X=[sb.tile("X" + u + "_%d" % z, [128, 64], BF16) for z in range(2)],
                TM=sb.tile("TM" + u, [128, 192], BF16),
                Wb=sb.tile("Wb" + u, [128, 64], BF16),
                Ub=sb.tile("Ub" + u, [128, 64], BF16),
                S=sb.tile("S" + u, [128, 64], F32),
                Sb=sb.tile("Sb" + u, [128, 64], BF16),
                Sg=sb.tile("Sg" + u, [128, 64], F32),
            )
        HS = [slice(0, 64), slice(64, 128)]

        def load_block(i, d, bi, z):
            st = U_[(i, d)]
            u = "%d_%d" % (i, d)
            cs = slice(i * 128, (i + 1) * 128)
            t0 = bi * TBS
            cx.dma('sp', st['AR'][z][:, :, 0:64], opT[d, 0, cs, t0:t0 + TBS].rearrange("p (c j) -> p c j", j=CH), ['opT'], ['AR%s_%d' % (u, z)])
            cx.dma('sp', st['AR'][z][:, :, 64:128], opT[d, 1, cs, t0:t0 + TBS].rearrange("p (c j) -> p c j", j=CH), ['opT'], ['AR%s_%d' % (u, z)])
            cx.dma('sp', st['BT'][z][:], opT[d, 2, cs, t0:t0 + TBS], ['opT'], ['BT%s_%d' % (u, z)])
            cx.dma('sp', st['KT'][z][:], opT[d, 3, cs, t0:t0 + TBS], ['opT'], ['KT%s_%d' % (u, z)])
            cx.dma('sp', st['VT'][z][:], vTs[cs, t0:t0 + TBS], ['vTs'], ['VT%s_%d' % (u, z)])
            cx.dma('sp', st['GM'][z][:], gams[d, cs, t0 // CH:t0 // CH + cpb], ['gams'], ['GM%s_%d' % (u, z)])

        def unit(i, d, bi, z, cc, pbank, pcol, first, cut):
            st = U_[(i, d)]
            u = "%d_%d" % (i, d)
            K = lambda nm: nm + u
            Kz = lambda nm: '%s%s_%d' % (nm, u, z)
            pk = 'ps%d' % pbank
            pf = ps[pbank][:, pcol * 256:(pcol + 1) * 256]
            pb_ = psb[pbank][:, pcol * 512:(pcol + 1) * 512]
            AR, BT, KT, VT, GM = st['AR'][z], st['BT'][z], st['KT'][z], st['VT'][z], st['GM'][z]
            M4, AA, X, TM, Wb, Ub, S, Sb, Sg = st['M4'], st['AA'], st['X'], st['TM'], st['Wb'], st['Ub'], st['S'], st['Sb'], st['Sg']
            tsl = slice(cc * CH, (cc + 1) * CH)
            if first:
                cx.op('pool', [], [K('S')], lambda e: e.memset(S[:], 0.0))
                cx.op('pool', [], [K('Sb')], lambda e: e.memset(Sb[:], 0.0))
            elif cut:
                cx.op('dve', [K('S'), 'link'], [K('S')], lambda e: e.tensor_scalar(S[:], S[:], linkt[:, 0:1], None, ALU.mult))
                cx.op('dve', [K('S')], [K('Sb')], lambda e: e.tensor_copy(Sb[:], S[:]))
            cx.op('pool', [K('S'), Kz('GM')], [K('Sg')], lambda e: e.tensor_scalar(Sg[:], S[:], GM[:, cc:cc + 1], None, ALU.mult))
            for H in HS:
                cx.op('pe', [Kz('BT'), Kz('AR')], [pk], lambda e: e.matmul(pf[H, 0:128], BT[H, tsl], AR[H, cc, :], start=True, stop=True))
                cx.op('pe', [Kz('KT'), Kz('AR')], [pk], lambda e: e.matmul(pf[H, 128:256], KT[H, tsl], AR[H, cc, :], start=True, stop=True))
            yield
            cx.op('dve', [pk, 'm4'], [pk, K('M4')], lambda e: e.tensor_tensor(M4[:], pf[:, 0:256], m4[:, d, :], ALU.mult))
            for H in HS:
                cx.op('pe', [Kz('AR'), Kz('BT')], [pk], lambda e: e.matmul(pf[H, 0:64], AR[H, cc, 0:64], BT[H, tsl], start=True, stop=True))
                for q, src in enumerate((BT, KT, VT)):
                    cx.op('pe', [Kz('BT'), Kz('KT'), Kz('VT'), 'identb'], [pk],
                          lambda e: e.transpose(pb_[H, 256 + q * 64:256 + (q + 1) * 64], src[H, tsl], identb[H, H]))
            yield
            cx.op('dve', [pk, 'mT'], [pk, K('AA0')], lambda e: e.tensor_tensor(AA[0][:, 64:128], pf[:, 0:64], mT[:, d, :], ALU.mult))
            cx.op('act', [pk], [pk, K('TM')], lambda e: e.activation(TM[:], pb_[:, 256:448], AF.Copy))
            cx.op('pool', [K('M4')], [K('AA0')], lambda e: e.tensor_copy(AA[0][:, 0:64], M4[:, 0:64]))
            cx.op('pool', [K('M4'), 'idh'], [K('X0')], lambda e: e.tensor_tensor(X[0][:], M4[:, 0:64], idh[:], ALU.add))
            for k in range(5):
                a, an = AA[k % 2], AA[(k + 1) % 2]
                ka, kan = K('AA%d' % (k % 2)), K('AA%d' % ((k + 1) % 2))
                xo, xn = X[k % 2], X[(k + 1) % 2]
                kxo, kxn = K('X%d' % (k % 2)), K('X%d' % ((k + 1) % 2))
                for H in HS:
                    if k < 4:
                        cx.op('pe', [ka], [pk], lambda e: e.matmul(pf[H, 0:64], a[H, 64:128], a[H, 0:64], start=True, stop=True))
                    cx.op('pe', [ka], [pk], lambda e: e.matmul(pf[H, 64:128], a[H, 0:64], a[H, 64:128], start=True, stop=True))
                    if k >= 1:
                        cx.op('pe', [ka, K('X%d' % ((k - 1) % 2))], [pk], lambda e: e.matmul(pf[H, 128:192], a[H, 64:128], X[(k - 1) % 2][H, :], start=True, stop=True))
                yield
                if k < 4:
                    cx.op('act', [pk], [pk, kan], lambda e: e.activation(an[:], pf[:, 0:128], AF.Copy))
                else:
                    cx.op('act', [pk], [pk, kan], lambda e: e.activation(an[:, 64:128], pf[:, 64:128], AF.Copy))
                if k >= 1:
                    cx.op('dve', [pk, K('X%d' % ((k - 1) % 2))], [pk, K('X%d' % (k % 2))],
                          lambda e: e.tensor_tensor(X[k % 2][:], pf[:, 128:192], X[(k - 1) % 2][:], ALU.add))
            a5 = AA[1]
            for H in HS:
                cx.op('pe', [K('AA1'), K('X0')], [pk], lambda e: e.matmul(pf[H, 0:64], a5[H, 64:128], X[0][H, :], start=True, stop=True))
                cx.op('pe', [Kz('AR'), K('Sb')], [pk], lambda e: e.matmul(pf[H, 64:128], AR[H, cc, 0:64], Sb[H, :], start=True, stop=False))
                cx.op('pe', [K('M4'), K('TM')], [pk], lambda e: e.matmul(pf[H, 64:128], M4[H, 128:192], TM[H, 128:192], start=False, stop=True))
            yield
            cx.op('dve', [pk, K('X0')], [pk, K('X1')], lambda e: e.tensor_tensor(X[1][:], pf[:, 0:64], X[0][:], ALU.add))
            cx.op('act', [pk], [pk, K('Wb')], lambda e: e.activation(Wb[:], pf[:, 64:128], AF.Copy))
            for H in HS:
                cx.op('pe', [K('X1'), K('Wb')], [pk], lambda e: e.matmul(pf[H, 0:64], X[1][H, :], Wb[H, :], start=True, stop=True))
            yield
            cx.op('dve', [pk], [pk, K('Ub')], lambda e: e.tensor_copy(Ub[:], pf[:, 0:64]))
            for H in HS:
                cx.op('pe', [K('Sb'), Kz('AR')], [pk], lambda e: e.matmul(pf[H, 0:64], Sb[H, :], AR[H, cc, 64:128], start=True, stop=False))
                cx.op('pe', [K('Ub'), K('M4')], [pk], lambda e: e.matmul(pf[H, 0:64], Ub[H, :], M4[H, 64:128], start=False, stop=False))
                cx.op('pe', [K('TM'), K('M4')], [pk], lambda e: e.matmul(pf[H, 0:64], TM[H, 128:192], M4[H, 192:256], start=False, stop=True))
                cx.op('pe', [K('TM'), K('Ub')], [pk], lambda e: e.matmul(pf[H, 64:128], TM[H, 0:64], Ub[H, :], start=True, stop=False))
                cx.op('pe', [K('TM')], [pk], lambda e: e.matmul(pf[H, 64:128], TM[H, 64:128], TM[H, 128:192], start=False, stop=True))
            yield
            cx.op('act', [pk], [pk, Kz('YB')], lambda e: e.activation(st['YB'][z][:, tsl], pf[:, 0:64], AF.Copy))
            cx.op('dve', [pk, Kz('GM'), K('Sg')], [pk, K('S')],
                  lambda e: e.scalar_tensor_tensor(S[:], pf[:, 64:128], GM[:, cc:cc + 1], Sg[:], ALU.mult, ALU.add))
            cx.op('act', [K('S')], [K('Sb')], lambda e: e.activation(Sb[:], S[:], AF.Copy))

        for (i, d) in units:
            load_block(i, d, 0 if d == 0 else nblk - 1, 0)
        for s in range(nblk):
            z = s % 2
            if s + 1 < nblk:
                for (i, d) in units:
                    load_block(i, d, s + 1 if d == 0 else nblk - 2 - s, (s + 1) % 2)
            for c in range(cpb):
                gens = []
                for n_, (i, d) in enumerate(units):
                    bi = s if d == 0 else nblk - 1 - s
                    cc = c if d == 0 else cpb - 1 - c
                    first = (s == 0 and c == 0)
                    cut = (c == 0 and s == NBS and nblk == 2 * NBS)
                    gens.append(unit(i, d, bi, z, cc, n_ // 2, n_ % 2, first, cut))
                alive = list(gens)
                while alive:
                    nxt = []
                    for g in alive:
                        try:
                            next(g)
                            nxt.append(g)
                        except StopIteration:
                            pass
                    alive = nxt
            for (i, d) in units:
                bi = s if d == 0 else nblk - 1 - s
                u = "%d_%d" % (i, d)
                cx.dma('sp', ysc[d, i * 128:(i + 1) * 128, bi * TBS:(bi + 1) * TBS], U_[(i, d)]['YB'][z][:], ['YB%s_%d' % (u, z)], ['ysc'])
        cx.barrier()
        sb.release()

    def phase_rwepi(l):
        TB = min(512, SEG)
        gnp = sb.tile("gnp", [128, 2, 6], F32)
        with nc.allow_non_contiguous_dma(reason="tiny param loads"):
            cx.dma('sp', gnp[:, 0, :], W['rw_lnx_g'][l].rearrange("(a p) -> p a", p=128), [], ['gnp'])
            cx.dma('sp', gnp[:, 1, :], W['rw_lnx_b'][l].rearrange("(a p) -> p a", p=128), [], ['gnp'])
        epsb = sb.tile("epsb", [128, 1], F32)
        cx.op('pool', [], ['epsb'], lambda e: e.memset(epsb[:], RW_GN_EPS))
        ya = [sb.tile("ya%d" % z, [128, TB], F32) for z in range(2)]
        yb = [sb.tile("yb%d" % z, [128, TB], F32) for z in range(2)]
        bn = [sb.tile("bn%d" % z, [128, TB], F32) for z in range(2)]
        gg = [sb.tile("gg%d" % z, [128, TB], F32) for z in range(2)]
        dd = [sb.tile("dd%d" % z, [128, TB], F32) for z in range(2)]
        sq = [sb.tile("sq%d" % z, [128, TB], F32) for z in range(2)]
        rs = [sb.tile("rs%d" % z, [128, TB], F32) for z in range(2)]
        oo = [sb.tile("oo%d" % z, [128, TB], BF16) for z in range(2)]
        n = 0
        for bk in range(T // TB):
            t0 = bk * TB
            for ct in range(6):
                z = n % 2
                n += 1
                cs = slice(ct * 128, (ct + 1) * 128)
                Z = lambda nm: '%s%d' % (nm, z)
                cx.dma('sp', ya[z][:], ysc[0, cs, t0:t0 + TB], ['ysc'], [Z('ya')])
                cx.dma('sp', yb[z][:], ysc[1, cs, t0:t0 + TB], ['ysc'], [Z('yb')])
                cx.dma('sp', bn[z][:], bons[cs, t0:t0 + TB], ['bons'], [Z('bn')])
                cx.dma('sp', gg[z][:], gTs[cs, t0:t0 + TB], ['gTs'], [Z('gg')])
                cx.op('pool', [Z('ya'), Z('yb')], [Z('ya')], lambda e: e.tensor_tensor(ya[z][:], ya[z][:], yb[z][:], ALU.add))
                cx.op('pe', [Z('ya'), 'blk'], [Z('ps')], lambda e: e.matmul(ps[z][:, 0:TB], blk[:], ya[z][:], start=True, stop=True))
                cx.op('dve', [Z('ps'), Z('ya')], [Z('dd')],
                      lambda e: e.scalar_tensor_tensor(dd[z][:], ps[z][:, 0:TB], -1.0 / 64, ya[z][:], ALU.mult, ALU.add))
                cx.op('act', [Z('dd')], [Z('sq')], lambda e: e.activation(sq[z][:], dd[z][:], AF.Square))
                cx.op('pe', [Z('sq'), 'blk'], ['ps%d' % (2 + z)], lambda e: e.matmul(ps[2 + z][:, 0:TB], blk[:], sq[z][:], start=True, stop=True))
                cx.op('act', ['ps%d' % (2 + z), 'epsb'], [Z('rs')],
                      lambda e: e.activation(rs[z][:], ps[2 + z][:, 0:TB], AF.Sqrt, bias=epsb[:, 0:1], scale=1.0 / 64))
                cx.op('dve', [Z('rs')], [Z('rs')], lambda e: e.reciprocal(rs[z][:], rs[z][:]))
                cx.op('pool', [Z('dd'), Z('rs')], [Z('dd')], lambda e: e.tensor_tensor(dd[z][:], dd[z][:], rs[z][:], ALU.mult))
                cx.op('dve', [Z('dd'), 'gnp'], [Z('dd')],
                      lambda e: e.tensor_scalar(dd[z][:], dd[z][:], gnp[:, 0, ct:ct + 1], gnp[:, 1, ct:ct + 1], ALU.mult, ALU.add))
                cx.op('pool', [Z('dd'), Z('bn')], [Z('dd')], lambda e: e.tensor_tensor(dd[z][:], dd[z][:], bn[z][:], ALU.add))
                cx.op('dve', [Z('dd'), Z('gg')], [Z('oo')], lambda e: e.tensor_tensor(oo[z][:], dd[z][:], gg[z][:], ALU.mult))
                cx.dma('sp', yT[cs, t0:t0 + TB], oo[z][:], [Z('oo')], ['yT'])
        cx.barrier()
        sb.release()

    x1s = nc.dram_tensor("x1s", [T, D], F32, kind=skind).ap()
    x1T = nc.dram_tensor("x1T", [D, T], BF16, kind=skind).ap()
    Gs = nc.dram_tensor("Gs", [T, NE], F32, kind=skind).ap()
    facc = nc.dram_tensor("facc", [T, D], F32, kind=skind).ap()
    x2s = nc.dram_tensor("x2s", [T, D], F32, kind=skind).ap()

    def layer_norm(pool_tiles, r, key_r, gB, bB, outt, key_o):
        stt, mv, rstd = pool_tiles
        for q in range(4):
            cx.op('dve', [key_r], ['lnst'], lambda e: e.bn_stats(stt[:, q, :], r[:, q * 512:(q + 1) * 512]))
        cx.op('dve', ['lnst'], ['lnmv'], lambda e: e.bn_aggr(mv[:], stt[:].rearrange("p a b -> p (a b)")))
        cx.op('act', ['lnmv', 'lneps'], ['lnrs'], lambda e: e.activation(rstd[:], mv[:, 1:2], AF.Sqrt, bias=lneps[:, 0:1]))
        cx.op('dve', ['lnrs'], ['lnrs'], lambda e: e.reciprocal(rstd[:], rstd[:]))
        cx.op('dve', [key_r, 'lnmv', 'lnrs'], [key_r], lambda e: e.tensor_scalar(r[:], r[:], mv[:, 0:1], rstd[:, 0:1], ALU.subtract, ALU.mult))
        cx.op('pool', [key_r, 'gB'], [key_r], lambda e: e.tensor_tensor(r[:], r[:], gB[:], ALU.mult))
        cx.op('dve', [key_r, 'bB'], [key_o], lambda e: e.tensor_tensor(outt[:], r[:], bB[:], ALU.add))

    lneps = nc.alloc_sbuf_tensor("lneps_sb", [128, 1], F32)
    cx.op('pool', [], ['lneps'], lambda e: e.memset(lneps[:], LN_EPS))

    def phase_outproj(l, xsrc):
        wo = sb.tile("wo", [128, 16, D], BF16)
        wov = W['w_out'][l].rearrange("(kc p) d -> p kc d", p=128)
        for hh in range(2):
            cx.dma('pool', wo[:, :, hh * 1024:(hh + 1) * 1024], wov[:, :, hh * 1024:(hh + 1) * 1024], [], ['wo'])
        gB = sb.tile("gB", [128, D], F32)
        bB = sb.tile("bB", [128, D], F32)
        cx.dma('sp', gB[:], W['ln1_g'][l].partition_broadcast(128), [], ['gB'])
        cx.dma('sp', bB[:], W['ln1_b'][l].partition_broadcast(128), [], ['bB'])
        rw = sb.tile("rw", [128, 16, NE], F32)
        cx.dma('sp', rw[:], W['router_w'].rearrange("(kc p) e -> p kc e", p=128), [], ['rw'])
        rb = sb.tile("rb", [128, NE], F32)
        cx.dma('sp', rb[:], W['router_bias'].partition_broadcast(128), [], ['rb'])
        yt = [sb.tile("yt%d" % z, [128, 16, 128], BF16) for z in range(2)]
        xt = [sb.tile("xt%d" % z, [128, D], F32) for z in range(2)]
        rr = [sb.tile("rr%d" % z, [128, D], F32) for z in range(2)]
        xo = [sb.tile("xo%d" % z, [128, D], F32) for z in range(2)]
        xTf = [sb.tile("xTf%d" % z, [128, 16, 128], F32) for z in range(2)]
        xTb = [sb.tile("xTb%d" % z, [128, 16, 128], BF16) for z in range(2)]
        stt = sb.tile("stt", [128, 4, 6], F32)
        mv = sb.tile("mv", [128, 2], F32)
        rstd = sb.tile("rstd", [128, 1], F32)
        sc = sb.tile("rsc", [128, NE], F32)
        sel = sb.tile("rsel", [128, NE], F32)
        tmp = sb.tile("rtmp", [128, NE], F32)
        msk = sb.tile("rmsk", [128, NE], F32)
        sm = sb.tile("rsm", [128, 16], F32)
        ing = sb.tile("ring", [128, 4], F32)
        Gt = [sb.tile("Gt%d" % z, [128, NE], F32) for z in range(2)]
        for nt in range(NT):
            t0 = nt * 128
            z = nt % 2
            Z = lambda nm: '%s%d' % (nm, z)
            cx.dma('sp', yt[z][:], yT[:, t0:t0 + 128].rearrange("(kc p) t -> p kc t", p=128), ['yT'], [Z('yt')])
            cx.dma('sp', xt[z][:], xsrc[t0:t0 + 128, :], ['xsrc'], [Z('xt')])
            for dc in range(4):
                pk = 'ps%d' % dc
                for kc in range(16):
                    cx.op('pe', [Z('yt'), 'wo'], [pk], lambda e: e.matmul(ps[dc][:], yt[z][:, kc, :], wo[:, kc, dc * 512:(dc + 1) * 512],
                                                                         start=(kc == 0), stop=(kc == 15)))
                cx.op('dve', [pk, Z('xt')], [pk, Z('rr')],
                      lambda e: e.scalar_tensor_tensor(rr[z][:, dc * 512:(dc + 1) * 512], xt[z][:, dc * 512:(dc + 1) * 512], ALPHA, ps[dc][:], ALU.mult, ALU.add))
            layer_norm((stt, mv, rstd), rr[z], Z('rr'), gB, bB, xo[z], Z('xo'))
            cx.dma('sp', x1s[t0:t0 + 128, :], xo[z][:], [Z('xo')], ['x1s'])
            for g4 in range(4):
                pk = 'ps%d' % (4 + g4 % 2)
                for q in range(4):
                    kc = g4 * 4 + q
                    cx.op('pe', [Z('xo'), 'ident'], [pk], lambda e: e.transpose(ps[4 + g4 % 2][:, q * 128:(q + 1) * 128], xo[z][:, kc * 128:(kc + 1) * 128], ident[:]))
                src = ps[4 + g4 % 2][:].rearrange("p (q t) -> p q t", q=4)
                cx.op('act', [pk], [pk, Z('xTf')], lambda e: e.activation(xTf[z][:, g4 * 4:(g4 + 1) * 4, :], src, AF.Copy))
            cx.op('pool', [Z('xTf')], [Z('xTb')], lambda e: e.tensor_copy(xTb[z][:], xTf[z][:]))
            cx.dma('sp', x1T[:, t0:t0 + 128].rearrange("(kc p) t -> p kc t", p=128), xTb[z][:], [Z('xTb')], ['x1T'])
            for kc in range(16):
                cx.op('pe', [Z('xTf'), 'rw'], ['ps6'], lambda e: e.matmul(ps[6][:, 0:NE], xTf[z][:, kc, :], rw[:, kc, :], start=(kc == 0), stop=(kc == 15)))
            cx.op('act', ['ps6'], ['ps6', 'rsc'], lambda e: e.activation(sc[:], ps[6][:, 0:NE], AF.Sigmoid))
            R = ['rsc', 'rsel', 'rtmp', 'rmsk', 'rsm', 'ring']
            cx.op('dve', ['rsc', 'rb'], ['rsel'], lambda e: e.tensor_tensor(sel[:], sc[:], rb[:], ALU.add))
            for g in range(4):
                sg = slice(4 * g, 4 * g + 4)
                cx.op('dve', R, R, lambda e: e.tensor_reduce(sm[:, g:g + 1], sel[:, sg], AX.X, ALU.max))
                cx.op('dve', R, R, lambda e: e.tensor_scalar(tmp[:, sg], sel[:, sg], sm[:, g:g + 1], -1e9, ALU.is_equal, ALU.mult))
                cx.op('dve', R, R, lambda e: e.tensor_tensor(tmp[:, sg], tmp[:, sg], sel[:, sg], ALU.add))
                cx.op('dve', R, R, lambda e: e.tensor_reduce(sm[:, 4 + g:5 + g], tmp[:, sg], AX.X, ALU.max))
            cx.op('dve', R, R, lambda e: e.tensor_tensor(sm[:, 8:12], sm[:, 0:4], sm[:, 4:8], ALU.add))
            cx.op('dve', R, R, lambda e: e.tensor_reduce(sm[:, 12:13], sm[:, 8:12], AX.X, ALU.max))
            cx.op('dve', R, R, lambda e: e.tensor_scalar(ing[:], sm[:, 8:12], sm[:, 12:13], None, ALU.is_equal))
            for g in range(4):
                sg = slice(4 * g, 4 * g + 4)
                cx.op('dve', R, R, lambda e: e.tensor_scalar(msk[:, sg], sel[:, sg], sm[:, 4 + g:5 + g], ing[:, g:g + 1], ALU.is_ge, ALU.mult))
            cx.op('dve', R, R, lambda e: e.tensor_tensor(tmp[:], sc[:], msk[:], ALU.mult))
            cx.op('dve', R, R, lambda e: e.tensor_reduce(sm[:, 13:14], tmp[:], AX.X, ALU.add))
            cx.op('dve', R, R, lambda e: e.reciprocal(sm[:, 14:15], sm[:, 13:14]))
            cx.op('dve', R, R + [Z('Gt')], lambda e: e.tensor_scalar(Gt[z][:], tmp[:], sm[:, 14:15], None, ALU.mult))
            cx.dma('sp', Gs[t0:t0 + 128, :], Gt[z][:], [Z('Gt')], ['Gs'])
        cx.barrier()
        sb.release()

    def phase_moe(l):
        TC = min(512, T)
        ntc = T // TC
        w1 = sb.tile("w1", [128, 16, DFF], BF16)
        w3 = sb.tile("w3", [128, 16, DFF], BF16)
        w2 = sb.tile("w2", [128, 8, D], BF16)
        xT = [sb.tile("mxT%d" % z, [128, 16, TC], BF16) for z in range(2)]
        hT = sb.tile("hT", [128, 8, TC], BF16)
        hs = [sb.tile("hs%d" % z, [128, TC], F32) for z in range(2)]
        Gt = sb.tile("mGt", [128, T // 128, NE], F32)
        for nt_ in range(T // 128):
            cx.dma('sp', Gt[:, nt_, :], Gs[nt_ * 128:(nt_ + 1) * 128, :], ['Gs'], ['mGt'])
        ac = [sb.tile("ac%d" % z, [128, D], F32) for z in range(3)]
        na = 0
        nx = 0
        for ex in range(NE):
            w1v = W['exp_w1'][l, ex].rearrange("(kc p) f -> p kc f", p=128)
            w3v = W['exp_w3'][l, ex].rearrange("(kc p) f -> p kc f", p=128)
            w2v = W['exp_w2'][l, ex].rearrange("(kc p) d -> p kc d", p=128)
            for hh in range(2):
                cx.dma('pool', w1[:, hh * 8:(hh + 1) * 8, :], w1v[:, hh * 8:(hh + 1) * 8, :], [], ['w1'])
                cx.dma('pool', w3[:, hh * 8:(hh + 1) * 8, :], w3v[:, hh * 8:(hh + 1) * 8, :], [], ['w3'])
            for hh in range(2):
                cx.dma('pool', w2[:, :, hh * 1024:(hh + 1) * 1024], w2v[:, :, hh * 1024:(hh + 1) * 1024], [], ['w2'])
            for tcn in range(ntc):
                t0 = tcn * TC
                z = nx % 2
                nx += 1
                XK = 'mxT%d' % z
                cx.dma('sp', xT[z][:], x1T[:, t0:t0 + TC].rearrange("(kc p) t -> p kc t", p=128), ['x1T'], [XK])
                for ft in range(8):
                    fs = slice(ft * 128, (ft + 1) * 128)
                    for kc in range(16):
                        cx.op('pe', ['w1', XK], ['ps0'], lambda e: e.matmul(ps[0][:, 0:TC], w1[:, kc, fs], xT[z][:, kc, :], start=(kc == 0), stop=(kc == 15)))
                    for kc in range(16):
                        cx.op('pe', ['w3', XK], ['ps1'], lambda e: e.matmul(ps[1][:, 0:TC], w3[:, kc, fs], xT[z][:, kc, :], start=(kc == 0), stop=(kc == 15)))
                    hz = ft % 2
                    cx.op('act', ['ps0'], ['ps0', 'hs%d' % hz], lambda e: e.activation(hs[hz][:], ps[0][:, 0:TC], AF.Silu))
                    cx.op('dve', ['ps1', 'hs%d' % hz], ['ps1', 'hT'], lambda e: e.tensor_tensor(hT[:, ft, :], ps[1][:, 0:TC], hs[hz][:], ALU.mult))
                for tt in range(TC // 128):
                    nt = (t0 // 128) + tt
                    az = na % 3
                    na += 1
                    AK = 'ac%d' % az
                    if ex > 0:
                        cx.dma('sp', ac[az][:], facc[nt * 128:(nt + 1) * 128, :], ['facc'], [AK])
                    for dc in range(4):
                        pk = 'ps%d' % (2 + dc)
                        for ft in range(8):
                            cx.op('pe', ['hT', 'w2'], [pk], lambda e: e.matmul(ps[2 + dc][:], hT[:, ft, tt * 128:(tt + 1) * 128], w2[:, ft, dc * 512:(dc + 1) * 512],
                                                                              start=(ft == 0), stop=(ft == 7)))
                        dsl = slice(dc * 512, (dc + 1) * 512)
                        if ex == 0:
                            cx.op('dve', [pk, 'mGt'], [pk, AK], lambda e: e.tensor_scalar(ac[az][:, dsl], ps[2 + dc][:], Gt[:, nt, ex:ex + 1], None, ALU.mult))
                        else:
                            cx.op('dve', [pk, 'mGt', AK], [pk, AK],
                                  lambda e: e.scalar_tensor_tensor(ac[az][:, dsl], ps[2 + dc][:], Gt[:, nt, ex:ex + 1], ac[az][:, dsl], ALU.mult, ALU.add))
                    cx.dma('sp', facc[nt * 128:(nt + 1) * 128, :], ac[az][:], [AK], ['facc'])
        cx.barrier()
        sb.release()

    def phase_ln2(l, dst):
        gB = sb.tile("gB2", [128, D], F32)
        bB = sb.tile("bB2", [128, D], F32)
        cx.dma('sp', gB[:], W['ln2_g'][l].partition_broadcast(128), [], ['gB'])
        cx.dma('sp', bB[:], W['ln2_b'][l].partition_broadcast(128), [], ['bB'])
        xt = [sb.tile("lxt%d" % z, [128, D], F32) for z in range(2)]
        ft_ = [sb.tile("lft%d" % z, [128, D], F32) for z in range(2)]
        xo = [sb.tile("lxo%d" % z, [128, D], F32) for z in range(2)]
        stt = sb.tile("stt2", [128, 4, 6], F32)
        mv = sb.tile("mv2", [128, 2], F32)
        rstd = sb.tile("rstd2", [128, 1], F32)
        for nt in range(NT):
            t0 = nt * 128
            z = nt % 2
            Z = lambda nm: '%s%d' % (nm, z)
            cx.dma('sp', xt[z][:], x1s[t0:t0 + 128, :], ['x1s'], [Z('lxt')])
            cx.dma('sp', ft_[z][:], facc[t0:t0 + 128, :], ['facc'], [Z('lft')])
            cx.op('dve', [Z('lxt'), Z('lft')], [Z('lft')], lambda e: e.scalar_tensor_tensor(ft_[z][:], xt[z][:], ALPHA, ft_[z][:], ALU.mult, ALU.add))
            layer_norm((stt, mv, rstd), ft_[z], Z('lft'), gB, bB, xo[z], Z('lxo'))
            cx.dma('sp', dst[t0:t0 + 128, :], xo[z][:], [Z('lxo')], ['dst'])
        cx.barrier()
        sb.release()

    layers = [0] if dbg in ('l0', 'nomoe') else [0, 1]
    for l in layers:
        xsrc = x_in if l == 0 else x2s
        phase_inproj(l, xsrc, pT)
        phase_rwprep(l)
        phase_rwscan(l)
        phase_rwepi(l)
        phase_lru(l)
        phase_att(l)
        phase_outproj(l, xsrc)
        if dbg != 'nomoe':
            phase_moe(l)
        phase_ln2(l, x2s if l == 0 else y_out)

    cx.finish()
    nc._used_w = list(W.keys())
    return nc


_CACHE = {}


def _run(SEG, x_cores, link_cores, weights, dbg=None):
    key = (SEG, dbg)
    if key not in _CACHE:
        _CACHE[key] = build(SEG, dbg)
    nc = _CACHE[key]
    ident = np.eye(128, dtype=np.float32)
    slopes = 2.0 ** (-8.0 * np.arange(1, 13, dtype=np.float64) / 12.0)
    dist = np.abs(np.arange(128)[:, None] - (np.arange(384)[None, :] - 128)).astype(np.float64)
    abias = np.where(dist[:, None, :] <= 128, -slopes[None, :, None] * dist[:, None, :], -30000.0).astype(np.float32)
    pj = (np.arange(128) % 64)[:, None]
    tt = np.arange(64)[None, :]
    sf, inf_ = (pj < tt), (pj <= tt)
    sb_, inb = (pj > tt), (pj >= tt)
    m4 = np.stack([np.concatenate([sf, inf_, sf, inf_], 1), np.concatenate([sb_, inb, sb_, inb], 1)], 1).astype(np.float32)
    mT = np.stack([sb_, sf], 1).astype(np.float32)
    idh = (pj == tt).astype(np.float32)
    blk = np.kron(np.eye(2), np.ones((64, 64))).astype(np.float32)
    in_maps = []
    for c in range(8):
        m = {"x": x_cores[c], "link": np.full((128, 1), link_cores[c], np.float32), "ident": ident, "abias": abias, "blk": blk, "m4": m4, "mT": mT, "idh": idh}
        for name in nc._used_w:
            m[name] = weights[name]
        in_maps.append(m)
    res = run_bass_kernel_spmd(nc, in_maps, core_ids=list(range(8)))
    return res.results


def kernel(**inp):
    xp = np.asarray(inp['x_prompt'], np.float32)
    xsm = np.asarray(inp['x_sample'], np.float32)
    SEG = xp.shape[1]
    assert xsm.shape[1] == 2 * SEG and xp.shape[0] == 8 and xsm.shape[0] == 4
    x_cores = [np.ascontiguousarray(xp[2 * c:2 * c + 2].reshape(2 * SEG, D)) for c in range(4)]
    x_cores += [np.ascontiguousarray(xsm[c]) for c in range(4)]
    link = [0.0] * 4 + [1.0] * 4
    w = {k: np.ascontiguousarray(np.asarray(v, np.float32)) for k, v in inp.items() if k not in ('x_prompt', 'x_sample')}
    res = _run(SEG, x_cores, link, w)
    yp = np.stack([res[c]["y"].reshape(2, SEG, D) for c in range(4)]).reshape(8, SEG, D)
    ys = np.stack([res[4 + c]["y"] for c in range(4)])
    return (yp.astype(np.float32), ys.astype(np.float32))
```
